# Optimizing a Trainium2 kernel written in Bass

```python
import jax
import jax.numpy as jnp
from jax import lax
import numpy as np

D_MODEL = 1024
BATCH = 1
SEQ = 16384
DEPTH = 2

GRID_W = 64
CTX_LEN = 256
EPS = 1e-6
HEAD_DIM = 64
N_Q_HEADS = 8
N_KV_HEADS = 2
Q_PER_KV = N_Q_HEADS // N_KV_HEADS
ATTN_WIDTH = N_Q_HEADS * HEAD_DIM
KV_WIDTH = N_KV_HEADS * HEAD_DIM
WINDOW = 128
ATTN_BLOCK = 128
ROPE_THETA = 10000.0
AXIS_DIM = HEAD_DIM // 2
HG_HEADS = 4
HG_K = 64
HG_V = 64
HG_WIDTH = HG_HEADS * HG_V
HG_CHUNK = 64
GM_GROUPS = 4
GM_DIM = 64
GM_WIDTH = GM_GROUPS * GM_DIM
GM_CHUNK = 128
MIX_WIDTH = ATTN_WIDTH + HG_WIDTH + GM_WIDTH
IN_SPLITS = (ATTN_WIDTH, KV_WIDTH, KV_WIDTH, HG_WIDTH, HG_WIDTH, HG_WIDTH, HG_WIDTH, HG_WIDTH, GM_WIDTH, GM_WIDTH)
IN_WIDTH = ATTN_WIDTH + 2 * KV_WIDTH + 5 * HG_WIDTH + 2 * GM_WIDTH
FFN_DIM = 2816
N_EXPERTS = 8
TOP_K = 2
EXPERT_DIM = 3584
N_DENSE = (DEPTH + 1) // 2
N_MOE = DEPTH // 2

kernel_name = 'hybrid_hgrn2_swa_gmlp_moe_dit_block'


def _rms(x):
    xf = x.astype(jnp.float32)
    return (xf * lax.rsqrt(jnp.mean(xf * xf, axis=-1, keepdims=True) + EPS)).astype(x.dtype)


def _heads(t, n):
    return t.reshape(t.shape[:-1] + (n, t.shape[-1] // n))


def _split_cols(p):
    out, start = [], 0
    for w in IN_SPLITS:
        out.append(p[..., start:start + w])
        start += w
    return out


def _axial_rope_tables(rows, dtype):
    row = jnp.repeat(jnp.arange(rows, dtype=jnp.float32), GRID_W, total_repeat_length=rows * GRID_W)
    col = jnp.tile(jnp.arange(GRID_W, dtype=jnp.float32), rows)
    inv_freq = ROPE_THETA ** (-jnp.arange(0, AXIS_DIM, 2, dtype=jnp.float32) / AXIS_DIM)
    ang_r = row[:, None] * inv_freq
    ang_c = col[:, None] * inv_freq
    return tuple(t.astype(dtype) for t in (jnp.cos(ang_r), jnp.sin(ang_r), jnp.cos(ang_c), jnp.sin(ang_c)))


def _rope_half(x, cos, sin):
    x1, x2 = jnp.split(x, 2, axis=-1)
    cos = cos[:, None, :]
    sin = sin[:, None, :]
    return jnp.concatenate([x1 * cos - x2 * sin, x1 * sin + x2 * cos], axis=-1)


def _apply_axial_rope(x, tables):
    cos_r, sin_r, cos_c, sin_c = tables
    return jnp.concatenate([_rope_half(x[..., :AXIS_DIM], cos_r, sin_r),
                            _rope_half(x[..., AXIS_DIM:], cos_c, sin_c)], axis=-1)


def _window_attention(q, k, v, k_ctx, v_ctx, sink):
    b, s = q.shape[:2]
    nb = s // ATTN_BLOCK
    qb = q.reshape(b, nb, ATTN_BLOCK, N_KV_HEADS, Q_PER_KV, HEAD_DIM) * (HEAD_DIM ** -0.5)
    pad = ((0, 0), (ATTN_BLOCK, ATTN_BLOCK), (0, 0), (0, 0))

    def band_blocks(t):
        tp = jnp.pad(t, pad).reshape(b, nb + 2, ATTN_BLOCK, N_KV_HEADS, HEAD_DIM)
        return jnp.concatenate([tp[:, :-2], tp[:, 1:-1], tp[:, 2:]], axis=2)

    kw, vw = band_blocks(k), band_blocks(v)
    s_win = jnp.einsum('bnqhgd,bnkhd->bnhgqk', qb, kw).astype(jnp.float32)
    a = jnp.arange(ATTN_BLOCK)[:, None]
    r = jnp.arange(3 * ATTN_BLOCK)[None, :]
    band = jnp.abs(a + ATTN_BLOCK - r) <= WINDOW
    key_pos = jnp.arange(nb)[:, None] * ATTN_BLOCK - ATTN_BLOCK + jnp.arange(3 * ATTN_BLOCK)[None, :]
    in_range = (key_pos >= 0) & (key_pos < s)
    valid = band[None] & in_range[:, None, :]
    s_win = jnp.where(valid[None, :, None, None], s_win, -jnp.inf)
    s_ctx = jnp.einsum('bnqhgd,bchd->bnhgqc', qb, k_ctx).astype(jnp.float32)
    s_sink = jnp.broadcast_to(sink.astype(jnp.float32).reshape(N_KV_HEADS, Q_PER_KV)[None, None, :, :, None, None],
                              s_win.shape[:-1] + (1,))
    p = jax.nn.softmax(jnp.concatenate([s_win, s_ctx, s_sink], axis=-1), axis=-1)
    n_win = 3 * ATTN_BLOCK
    n_ctx = k_ctx.shape[1]
    p_win = p[..., :n_win].astype(v.dtype)
    p_ctx = p[..., n_win:n_win + n_ctx].astype(v.dtype)
    o = (jnp.einsum('bnhgqk,bnkhd->bnqhgd', p_win, vw)
         + jnp.einsum('bnhgqc,bchd->bnqhgd', p_ctx, v_ctx))
    return o.reshape(b, s, ATTN_WIDTH)


def _context_attention(q, k, v, sink):
    b, n = q.shape[:2]
    qg = q.reshape(b, n, N_KV_HEADS, Q_PER_KV, HEAD_DIM) * (HEAD_DIM ** -0.5)
    sc = jnp.einsum('bqhgd,bkhd->bhgqk', qg, k).astype(jnp.float32)
    sk = jnp.broadcast_to(sink.astype(jnp.float32).reshape(N_KV_HEADS, Q_PER_KV)[None, :, :, None, None],
                          sc.shape[:-1] + (1,))
    p = jax.nn.softmax(jnp.concatenate([sc, sk], axis=-1), axis=-1)[..., :-1]
    o = jnp.einsum('bhgqk,bkhd->bqhgd', p.astype(v.dtype), v)
    return o.reshape(b, n, ATTN_WIDTH)


def _to_chunks(t, chunk):
    b, n, h, d = t.shape
    return t.reshape(b, n // chunk, chunk, h, d).transpose(1, 0, 3, 2, 4)


def _hgrn_scan(q, k, v, log_f, s0):
    qc, kc, vc, gc = (_to_chunks(t.astype(jnp.float32), HG_CHUNK) for t in (q, k, v, log_f))
    tri = jnp.tril(jnp.ones((HG_CHUNK, HG_CHUNK), dtype=bool))[:, :, None]

    def step(state, blk):
        qb, kb, vb, gb = blk
        cum = jnp.cumsum(gb, axis=2)
        o_inter = jnp.einsum('bhtk,bhkv->bhtv', qb * jnp.exp(cum), state)
        diff = cum[:, :, :, None, :] - cum[:, :, None, :, :]
        decay = jnp.exp(jnp.where(tri, diff, -jnp.inf))
        scores = jnp.einsum('bhtk,bhsk,bhtsk->bhts', qb, kb, decay)
        o_intra = jnp.einsum('bhts,bhsv->bhtv', scores, vb)
        last = cum[:, :, -1:, :]
        new_state = (jnp.exp(last[:, :, 0, :])[..., None] * state
                     + jnp.einsum('bhsk,bhsv->bhkv', kb * jnp.exp(last - cum), vb))
        return new_state, o_inter + o_intra

    final, out = lax.scan(step, s0, (qc, kc, vc, gc))
    b, n, h = q.shape[:3]
    out = out.transpose(1, 0, 3, 2, 4).reshape(b, n, h, v.shape[-1])
    return out.astype(v.dtype), final


def _hgrn_direction(q, f_logit, lower_bound, v, s0):
    f = lower_bound + (1.0 - lower_bound) * jax.nn.sigmoid(f_logit.astype(jnp.float32))
    return _hgrn_scan(q, 1.0 - f, v, jnp.log(f), s0)


def _hgrn_bidir(hq, hff, hfb, hi, lb_f, lb_b, s0_f, s0_b):
    q = jax.nn.silu(_heads(hq, HG_HEADS)) * (HG_K ** -0.5)
    v = _heads(hi, HG_HEADS)
    o_f, st_f = _hgrn_direction(q, _heads(hff, HG_HEADS), lb_f, v, s0_f)
    o_b, st_b = _hgrn_direction(jnp.flip(q, 1), jnp.flip(_heads(hfb, HG_HEADS), 1), lb_b, jnp.flip(v, 1), s0_b)
    return o_f + jnp.flip(o_b, 1), st_f, st_b


def _hgrn_out(o, g, gain):
    y = _rms(o) * gain * jax.nn.silu(_heads(g, HG_HEADS))
    return y.reshape(y.shape[:-2] + (HG_WIDTH,))


def _chunk_gmlp(u, v, w_s, b_s, norm_gain):
    b, n, _ = u.shape
    nc = n // GM_CHUNK
    u = jax.nn.gelu(u, approximate=False)
    v = _rms(_heads(jax.nn.gelu(v, approximate=False), GM_GROUPS)) * norm_gain.reshape(GM_GROUPS, GM_DIM)
    vc = v.reshape(b, nc, GM_CHUNK, GM_GROUPS, GM_DIM)
    mixed = jnp.einsum('gts,bnsgc->bntgc', w_s, vc) + b_s.T[:, :, None]
    return u * mixed.reshape(b, n, GM_WIDTH)


def _swiglu(h, w_gate, w_up, w_down):
    return (jax.nn.silu(h @ w_gate) * (h @ w_up)) @ w_down


def _moe(h, router, w_gate, w_up, w_down):
    shape = h.shape
    t = h.reshape(-1, shape[-1])
    logits = (t @ router).astype(jnp.float32)
    top_val, top_idx = lax.top_k(logits, TOP_K)
    top_w = jax.nn.softmax(top_val, axis=-1)
    gate = jnp.sum(jax.nn.one_hot(top_idx, N_EXPERTS, dtype=jnp.float32) * top_w[..., None], axis=1).astype(t.dtype)
    out = jnp.zeros_like(t)
    for e in range(N_EXPERTS):
        out = out + gate[:, e:e + 1] * _swiglu(t, w_gate[e], w_up[e], w_down[e])
    return out.reshape(shape)


def _channel_mixer(h, layer, ffn_w_gate, ffn_w_up, ffn_w_down, moe_router, moe_w_gate, moe_w_up, moe_w_down):
    i = layer // 2
    if layer % 2 == 0:
        return _swiglu(h, ffn_w_gate[i], ffn_w_up[i], ffn_w_down[i])
    return _moe(h, moe_router[i], moe_w_gate[i], moe_w_up[i], moe_w_down[i])


def setup_inputs(seed: int = 0) -> dict:
    key = jax.random.key(seed)
    ks = jax.random.split(key, 24)
    D = D_MODEL

    def nrm(k, shape, s):
        return jax.random.normal(k, shape, jnp.float32) * s

    return {
        'x': nrm(ks[0], (BATCH, SEQ, D), 1.0),
        'c': nrm(ks[1], (BATCH, D), 1.0),
        'ctx': nrm(ks[2], (BATCH, CTX_LEN, D), 1.0),
        'c_ctx': nrm(ks[3], (D,), 1.0),
        'w_ada': nrm(ks[4], (DEPTH, D, 6 * D), 0.5 * D ** -0.5),
        'b_ada': nrm(ks[5], (DEPTH, 6 * D), 0.02),
        'w_in': nrm(ks[6], (DEPTH, D, IN_WIDTH), D ** -0.5),
        'w_out': nrm(ks[7], (DEPTH, MIX_WIDTH, D), MIX_WIDTH ** -0.5),
        'q_norm_gain': 1.0 + nrm(ks[8], (DEPTH, HEAD_DIM), 0.02),
        'k_norm_gain': 1.0 + nrm(ks[9], (DEPTH, HEAD_DIM), 0.02),
        'attn_sink': nrm(ks[10], (DEPTH, N_Q_HEADS), 0.5),
        'hgrn_lower_bound': nrm(ks[11], (2, DEPTH, HG_WIDTH), 1.0),
        'hgrn_out_gain': 1.0 + nrm(ks[12], (DEPTH, HG_V), 0.02),
        'gmlp_w_s': nrm(ks[13], (DEPTH, GM_GROUPS, GM_CHUNK, GM_CHUNK), GM_CHUNK ** -0.5),
        'gmlp_b_s': 1.0 + nrm(ks[14], (DEPTH, GM_GROUPS, GM_CHUNK), 0.02),
        'gmlp_norm_gain': 1.0 + nrm(ks[15], (DEPTH, GM_WIDTH), 0.02),
        'ffn_w_gate': nrm(ks[16], (N_DENSE, D, FFN_DIM), D ** -0.5),
        'ffn_w_up': nrm(ks[17], (N_DENSE, D, FFN_DIM), D ** -0.5),
        'ffn_w_down': nrm(ks[18], (N_DENSE, FFN_DIM, D), FFN_DIM ** -0.5),
        'moe_router': nrm(ks[19], (N_MOE, D, N_EXPERTS), D ** -0.5),
        'moe_w_gate': nrm(ks[20], (N_MOE, N_EXPERTS, D, EXPERT_DIM), D ** -0.5),
        'moe_w_up': nrm(ks[21], (N_MOE, N_EXPERTS, D, EXPERT_DIM), D ** -0.5),
        'moe_w_down': nrm(ks[22], (N_MOE, N_EXPERTS, EXPERT_DIM, D), EXPERT_DIM ** -0.5),
    }


def reference(x, c, ctx, c_ctx, w_ada, b_ada, w_in, w_out, q_norm_gain, k_norm_gain, attn_sink,
              hgrn_lower_bound, hgrn_out_gain, gmlp_w_s, gmlp_b_s, gmlp_norm_gain,
              ffn_w_gate, ffn_w_up, ffn_w_down, moe_router, moe_w_gate, moe_w_up, moe_w_down):
    bsz, n_lat = x.shape[:2]
    rows = n_lat // GRID_W
    rope = _axial_rope_tables(rows, x.dtype)
    lb_soft = jax.nn.softmax(hgrn_lower_bound.astype(jnp.float32), axis=1)
    lower_bounds = jnp.cumsum(lb_soft, axis=1) - lb_soft[:, :1]
    zero_state = jnp.zeros((bsz, HG_HEADS, HG_K, HG_V), jnp.float32)

    for layer in range(DEPTH):
        last = layer == DEPTH - 1
        mod = jax.nn.silu(c) @ w_ada[layer] + b_ada[layer]
        mod_c = jax.nn.silu(c_ctx) @ w_ada[layer] + b_ada[layer]
        sh_a, sc_a, g_a, sh_f, sc_f, g_f = jnp.split(mod[:, None, :], 6, axis=-1)
        csh_a, csc_a, cg_a, csh_f, csc_f, cg_f = jnp.split(mod_c, 6)

        h = _rms(x) * (1.0 + sc_a) + sh_a
        hc = _rms(ctx) * (1.0 + csc_a) + csh_a
        aq, ak, av, hq, hff, hfb, hi, hg, gu, gv = _split_cols(h @ w_in[layer])
        caq, cak, cav, chq, chff, chfb, chi, chg, cgu, cgv = _split_cols(hc @ w_in[layer])

        qg, kg = q_norm_gain[layer], k_norm_gain[layer]
        q = _apply_axial_rope(_rms(_heads(aq, N_Q_HEADS)) * qg, rope)
        k = _apply_axial_rope(_rms(_heads(ak, N_KV_HEADS)) * kg, rope)
        v = _heads(av, N_KV_HEADS)
        k_c = _rms(_heads(cak, N_KV_HEADS)) * kg
        v_c = _heads(cav, N_KV_HEADS)
        attn_lat = _window_attention(q, k, v, k_c, v_c, attn_sink[layer])

        lb_f = lower_bounds[0, layer].reshape(HG_HEADS, HG_K)
        lb_b = lower_bounds[1, layer].reshape(HG_HEADS, HG_K)
        o_hc, st_f, st_b = _hgrn_bidir(chq, chff, chfb, chi, lb_f, lb_b, zero_state, zero_state)
        o_hl, _, _ = _hgrn_bidir(hq, hff, hfb, hi, lb_f, lb_b, st_f, st_b)
        hg_lat = _hgrn_out(o_hl, hg, hgrn_out_gain[layer])

        gm_lat = _chunk_gmlp(gu, gv, gmlp_w_s[layer], gmlp_b_s[layer], gmlp_norm_gain[layer])

        mix = jnp.concatenate([attn_lat, hg_lat, gm_lat], axis=-1)
        x = x + g_a * (mix @ w_out[layer])
        if not last:
            q_c = _rms(_heads(caq, N_Q_HEADS)) * qg
            attn_ctx = _context_attention(q_c, k_c, v_c, attn_sink[layer])
            hg_ctx = _hgrn_out(o_hc, chg, hgrn_out_gain[layer])
            gm_ctx = _chunk_gmlp(cgu, cgv, gmlp_w_s[layer], gmlp_b_s[layer], gmlp_norm_gain[layer])
            mix_c = jnp.concatenate([attn_ctx, hg_ctx, gm_ctx], axis=-1)
            ctx = ctx + cg_a * (mix_c @ w_out[layer])

        h = _rms(x) * (1.0 + sc_f) + sh_f
        x = x + g_f * _channel_mixer(h, layer, ffn_w_gate, ffn_w_up, ffn_w_down,
                                     moe_router, moe_w_gate, moe_w_up, moe_w_down)
        if not last:
            hc = _rms(ctx) * (1.0 + csc_f) + csh_f
            ctx = ctx + cg_f * _channel_mixer(hc, layer, ffn_w_gate, ffn_w_up, ffn_w_down,
                                              moe_router, moe_w_gate, moe_w_up, moe_w_down)
    return x
```

```python
import numpy as np
import ml_dtypes
from contextlib import ExitStack
import concourse.bass as bass
import concourse.mybir as mybir
from concourse.bass_utils import run_bass_kernel_spmd

F32 = mybir.dt.float32
BF16 = mybir.dt.bfloat16
AF = mybir.ActivationFunctionType
ALU = mybir.AluOpType
AX = mybir.AxisListType

NCORES = 8
D = 1024
SEQ = 16384
TOK = SEQ // NCORES
NT = TOK // 128
NCT = 2
NTT = NT + NCT
EPS = 1e-6
IN_W = 2560
FFN_DIM = 2816
EXPERT_DIM = 3584
N_EXP = 8
HC = 32
NCH = 128 // HC


import types


def _freeze(fn):
    if fn.__closure__ is None:
        return fn
    cells = []
    for c in fn.__closure__:
        try:
            cells.append(types.CellType(c.cell_contents))
        except ValueError:
            cells.append(c)
    return types.FunctionType(fn.__code__, fn.__globals__, fn.__name__, fn.__defaults__, tuple(cells))


class Buf:
    __slots__ = ("name", "last_w", "readers")

    def __init__(self, name=""):
        self.name = name
        self.last_w = None
        self.readers = []


class Src:
    def __init__(self, prog, name, step):
        self.name = name
        self.step = step
        self.sem = prog.stack.enter_context(prog.nc.semaphore(name))
        self.count = 0


class Eng:
    def __init__(self, prog, name, eng):
        self.name = name
        self.eng = eng
        self.src = Src(prog, "s_" + name, 1)
        self.stream = []
        self.waited = {}
        self.pending = False


class Prog:
    def __init__(self):
        self.nc = bass.Bass("TRN2", target_bir_lowering=False)
        self.stack = ExitStack()
        nc = self.nc
        self.stack.enter_context(nc.allow_low_precision("bf16 matmul operands, fp32 accumulation"))
        self.stack.enter_context(nc.allow_non_contiguous_dma("layout"))
        self.E = {}
        for name, eng in (("pe", nc.tensor), ("act", nc.scalar), ("dve", nc.vector),
                          ("pool", nc.gpsimd), ("sp", nc.sync)):
            self.E[name] = Eng(self, name, eng)
        self.dsems = {}
        self.nbuf = 0
        self.scopes = []

    def sbuf(self, name, shape, dtype):
        st = self.scopes[-1] if self.scopes else self.stack
        return st.enter_context(self.nc.sbuf_tensor("sb_" + name, list(shape), dtype))

    def psum(self, name, shape, dtype):
        return self.stack.enter_context(self.nc.psum_tensor(name, list(shape), dtype))

    def dram(self, name, shape, dtype, kind="Internal"):
        return self.nc.dram_tensor(name, list(shape), dtype, kind=kind)

    def push_scope(self):
        self.scopes.append(ExitStack())

    def pop_scope(self):
        self.barrier()
        self.scopes.pop().close()

    def buf(self, name=""):
        self.nbuf += 1
        return Buf(name or f"b{self.nbuf}")

    def bufs(self, n, name=""):
        return [self.buf(f"{name}{i}") for i in range(n)]

    def dsem(self, name):
        if name not in self.dsems:
            self.dsems[name] = Src(self, "d_" + name, 16)
        return self.dsems[name]

    def _deps(self, e, reads, writes, own_src=None):
        deps = {}

        def add(d, raw=False):
            if d is None:
                return
            src, tick = d
            if src is own_src:
                return
            if src is e.src:
                if not raw or tick > src.count:
                    return
            if deps.get(src, 0) < tick:
                deps[src] = tick

        for b in reads:
            add(b.last_w, raw=True)
        for b in writes:
            add(b.last_w)
            for r in b.readers:
                add(r)
        out = []
        for src, tick in deps.items():
            if e.waited.get(src, 0) >= tick:
                continue
            assert src.count >= tick, f"wait on future tick {src.name} {tick} > {src.count}"
            e.waited[src] = tick
            out.append((src.sem, tick))
        return out

    def op(self, en, fn, reads=(), writes=(), mark=True):
        e = self.E[en]
        fn = _freeze(fn)
        waits = self._deps(e, reads, writes)
        if mark:
            e.src.count += 1
            tick = e.src.count
            e.pending = False
        else:
            tick = e.src.count + 1
            e.pending = True
        sem = e.src.sem

        def emit(eng, waits=waits, fn=fn, mark=mark, sem=sem):
            for s, t in waits:
                eng.wait_ge(s, t)
            ins = fn(eng)
            if mark:
                ins.then_inc(sem, 1)

        e.stream.append(emit)
        for b in reads:
            b.readers.append((e.src, tick))
        for b in writes:
            b.last_w = (e.src, tick)
            b.readers = []

    def dma(self, qn, ds, out, in_, reads=(), writes=()):
        e = self.E[qn]
        d = self.dsem(ds) if isinstance(ds, str) else ds
        waits = self._deps(e, reads, writes, own_src=d)
        d.count += 16
        tick = d.count

        def emit(eng, waits=waits, out=out, in_=in_, sem=d.sem):
            for s, t in waits:
                eng.wait_ge(s, t)
            eng.dma_start(out=out, in_=in_).then_inc(sem, 16)

        e.stream.append(emit)
        for b in reads:
            b.readers.append((d, tick))
        for b in writes:
            b.last_w = (d, tick)
            b.readers = []

    def barrier(self):
        for e in self.E.values():
            if e.pending:
                raise RuntimeError(f"engine {e.name} has an unmarked trailing instruction")
        srcs = [e.src for e in self.E.values()] + list(self.dsems.values())
        for e in self.E.values():
            waits = []
            for s in srcs:
                if s is e.src or s.count == 0:
                    continue
                if e.waited.get(s, 0) >= s.count:
                    continue
                e.waited[s] = s.count
                waits.append((s.sem, s.count))

            def emit(eng, waits=waits):
                for s, t in waits:
                    eng.wait_ge(s, t)

            if waits:
                e.stream.append(emit)

    def finish(self):
        self.barrier()
        nc = self.nc
        with nc.Block() as block:
            @block.tensor
            def _(eng):
                for f in self.E["pe"].stream:
                    f(eng)

            @block.scalar
            def _(eng):
                for f in self.E["act"].stream:
                    f(eng)

            @block.vector
            def _(eng):
                for f in self.E["dve"].stream:
                    f(eng)

            @block.gpsimd
            def _(eng):
                for f in self.E["pool"].stream:
                    f(eng)

            @block.sync
            def _(eng):
                for f in self.E["sp"].stream:
                    f(eng)
        while self.scopes:
            self.scopes.pop().close()
        self.stack.close()
        return nc


def _consts():
    bf = ml_dtypes.bfloat16
    s = np.arange(128)
    same = (s[:, None] // HC) == (s[None, :] // HC)
    i = (s % HC)[:, None]
    j = (s % HC)[None, :]
    hm = HC // 2
    mrel = np.zeros((2, 128, 128), np.float32)
    mrel[0] = same * ((i <= j).astype(np.float32) - (i <= hm - 1).astype(np.float32))
    mrel[1] = same * ((i >= j).astype(np.float32) - (i >= hm).astype(np.float32))
    ind = np.zeros((2, 128, 16), np.float32)
    for c in range(NCH):
        inc = (s // HC == c)
        ii = s % HC
        ind[0, :, 3 * c + 0] = inc & (ii <= hm - 1)
        ind[0, :, 3 * c + 1] = inc
        ind[0, :, 3 * c + 2] = inc & (ii > hm - 1)
        ind[1, :, 3 * c + 0] = inc & (ii >= hm)
        ind[1, :, 3 * c + 1] = inc
        ind[1, :, 3 * c + 2] = inc & (ii < hm)
    hmask = np.zeros((2, 128, 128), np.float32)
    hmask[0] = same & (i <= j)
    hmask[1] = same & (i >= j)
    ident = np.eye(128, dtype=np.float32)
    return dict(ident=ident.astype(bf), identf=ident, mrel=mrel, ind=ind, hmask=hmask)


def _rope_tables(core):
    inv = (10000.0 ** (-np.arange(0, 32, 2, dtype=np.float32) / 32)).astype(np.float32)
    out = np.zeros((128, NTT, 2, 64), np.float32)
    p = np.arange(128)
    for t in range(NT):
        tok = core * TOK + t * 128 + p
        row = (tok // 64).astype(np.float32)
        col = (tok % 64).astype(np.float32)
        ar = row[:, None] * inv[None, :]
        ac = col[:, None] * inv[None, :]
        cr, sr, cc, sc = np.cos(ar), np.sin(ar), np.cos(ac), np.sin(ac)
        out[:, t, 0, 0:16] = cr
        out[:, t, 0, 16:32] = cr
        out[:, t, 0, 32:48] = cc
        out[:, t, 0, 48:64] = cc
        out[:, t, 1, 0:16] = -sr
        out[:, t, 1, 16:32] = sr
        out[:, t, 1, 32:48] = -sc
        out[:, t, 1, 48:64] = sc
    out[:, NT:, 0, :] = 1.0
    return out


def _amask(core):
    r = np.arange(128)[:, None]
    a = np.arange(128)[None, :]
    mp = (r >= a).astype(np.float32)
    mn = (r <= a).astype(np.float32)
    out = np.stack([mp * (0.0 if core == 0 else 1.0), mp, mn * (0.0 if core == NCORES - 1 else 1.0), mn])
    return out.astype(ml_dtypes.bfloat16)


def build(kind, layer):
    A = kind == "A"
    last = layer == 1
    moe = layer % 2 == 1
    ctx_full = (not A) and (not last)
    P = Prog()
    nc = P.nc
    op, dma = P.op, P.dma

    def din(name, shape, dt=F32):
        return P.dram(name, shape, dt, kind="ExternalInput")

    def dout(name, shape, dt=F32):
        return P.dram(name, shape, dt, kind="ExternalOutput")

    xin = din("xin", [TOK, D])
    ctxin = din("ctxin", [NCT * 128, D])
    cvec = din("cvec", [2, D])
    w_ada = din("w_ada", [D, 6 * D])
    b_ada = din("b_ada", [6 * D])
    w_in = din("w_in", [D, IN_W])
    qkgain = din("qkgain", [2, 64])
    lbnd = din("lbnd", [2, 2, 256])
    ident_d = din("ident", [128, 128], BF16)
    identf_d = din("identf", [128, 128])
    mrel_d = din("mrel", [2, 128, 128])
    ind_d = din("ind", [2, 128, 16])
    rope_d = din("rope", [128, NTT, 2, 64])
    if A:
        kb_o = dout("kb", [2, 128, 128], BF16)
        vb_o = dout("vb", [2, 128, 128], BF16)
        ab_o = dout("ab", [128, 2, 2, 65])
    else:
        hmask_d = din("hmask", [2, 128, 128])
        amask_d = din("amask", [4, 128, 128], BF16)
        khalo = din("khalo", [2, 128, 128], BF16)
        vhalo = din("vhalo", [2, 128, 128], BF16)
        ab_all = din("ab_all", [128, NCORES, 2, 2, 65])
        rmask = din("rmask", [128, 2, NCORES])
        w_out = din("w_out", [D, D])
        sink = din("sink", [8])
        hgain = din("hgain", [64])
        gm_ws = din("gm_ws", [4, 128, 128])
        gm_bs = din("gm_bs", [4, 128])
        gm_gain = din("gm_gain", [256])
        if moe:
            router = din("router", [D, N_EXP])
            wg = din("wg", [N_EXP, D, EXPERT_DIM])
            wu = din("wu", [N_EXP, D, EXPERT_DIM])
            wd = din("wd", [N_EXP, EXPERT_DIM, D])
        else:
            wg = din("wg", [1, D, FFN_DIM])
            wu = din("wu", [1, D, FFN_DIM])
            wd = din("wd", [1, FFN_DIM, D])
        xout = dout("xout", [TOK, D])
        if ctx_full:
            ctxout = dout("ctxout", [NCT * 128, D])
    modd = P.dram("modd", [2, 6 * D], F32)

    def dbgdump(name, ap, shape, dt, reads):
        if not DBG:
            return
        t_ = dout("dbg_" + name, shape, dt)
        dma("sp", "dbgd", t_.ap(), ap, reads=reads)

    PSF = [P.psum(f"psf{i}", [128, 512], F32) for i in range(6)]
    PSFb = P.bufs(6, "psf")
    PST = [P.psum(f"pst{i}", [128, 1024], BF16) for i in range(2)]
    PSTb = P.bufs(2, "pst")
    rr = {"f": 0, "t": 0}

    def psf():
        i = rr["f"] % 6
        rr["f"] += 1
        return PSF[i], PSFb[i]

    def pst():
        i = rr["t"] % 2
        rr["t"] += 1
        return PST[i], PSTb[i]

    ident = P.sbuf("ident_sb", [128, 128], BF16)
    identf = P.sbuf("identf_sb", [128, 128], F32)
    mrel = P.sbuf("mrel_sb", [128, 2, 128], F32)
    ind = P.sbuf("ind_sb", [128, 2, 16], F32)
    cb = P.buf("consts")
    dma("sp", "c0", ident[:, :], ident_d[:, :], writes=[cb])
    dma("sp", "c0", identf[:, :], identf_d[:, :], writes=[cb])
    dma("sp", "c0", mrel[:, :, :], mrel_d.ap().rearrange("d s t -> s d t"), writes=[cb])
    dma("sp", "c0", ind[:, :, :], ind_d.ap().rearrange("d s t -> s d t"), writes=[cb])

    nmod = 2 if A else 6
    P.push_scope()
    c_sb = P.sbuf("c_sb", [128, 2, 8], F32)
    c_b = P.buf()
    dma("sp", "m0", c_sb[:, 0, :], cvec[0, :].rearrange("(kc p) -> p kc", p=128), writes=[c_b])
    dma("sp", "m0", c_sb[:, 1, :], cvec[1, :].rearrange("(kc p) -> p kc", p=128), writes=[c_b])
    csl = P.sbuf("csl", [128, 2, 8], F32)
    csl_b = P.buf()
    op("act", lambda e: e.activation(csl[:, :, :], c_sb[:, :, :], AF.Silu), reads=[c_b], writes=[csl_b])
    cbc = P.sbuf("cbc", [128, 2, 8, 128], BF16)
    cbc_b = P.buf()
    op("dve", lambda e: e.tensor_copy(cbc[:, :, :, :], csl[:, :, :][:, :, :, None].broadcast_to([128, 2, 8, 128])),
       reads=[csl_b], writes=[cbc_b])
    wch = [P.sbuf(f"wada{i}", [128, 8, 512], BF16) for i in range(2)]
    wch_b = P.bufs(2, "wada")
    bch = [P.sbuf(f"bada{i}", [128, 512], F32) for i in range(2)]
    bch_b = P.bufs(2, "bada")
    mres = [P.sbuf(f"mres{i}", [1, 2, 512], F32) for i in range(2)]
    mres_b = P.bufs(2, "mres")
    modd_b = P.buf("modd")
    w_ada_v = w_ada.ap().rearrange("(kc p) n -> p kc n", p=128)
    for n in range(nmod * 2):
        s = n % 2
        dma("pool", f"wada{s}", wch[s][:, :, :], w_ada_v[:, :, n * 512:(n + 1) * 512], writes=[wch_b[s]])
        dma("sp", f"bada{s}", bch[s][0:1, :], b_ada[n * 512:(n + 1) * 512][None, :], writes=[bch_b[s]])
        for v in range(2):
            ps, psb = psf()
            for kc in range(8):
                op("pe", lambda e, ps=ps, v=v, kc=kc, s=s: e.matmul(ps[:, :], cbc[:, v, kc, :], wch[s][:, kc, :],
                                                                  start=(kc == 0), stop=(kc == 7)),
                   reads=[cbc_b, wch_b[s]], writes=[psb], mark=(kc == 7))
            op("dve", lambda e, ps=ps, v=v, s=s: e.tensor_tensor(mres[s][0:1, v, :], ps[0:1, :], bch[s][0:1, :], op=ALU.add),
               reads=[psb, bch_b[s]], writes=[mres_b[s]])
        dma("sp", f"mres{s}", modd.ap()[:, n * 512:(n + 1) * 512][None, :, :], mres[s][0:1, :, :],
            reads=[mres_b[s]], writes=[modd_b])
    P.pop_scope()

    def load_mod(dst, which, chunk, plus1, buf):
        dma("sp", "modld", dst[:, :], modd.ap()[which, chunk * D:(chunk + 1) * D][None, :].broadcast_to([128, D]),
            reads=[modd_b], writes=[buf])
        if plus1:
            op("pool", lambda e: e.tensor_scalar_add(dst[:, :], dst[:, :], 1.0), reads=[buf], writes=[buf])

    def rstd_from_ss(ss_ap, out_ap, dim, rb, wb):
        op("act", lambda e: e.activation(out_ap, ss_ap, AF.Ln, bias=EPS, scale=1.0 / dim), reads=rb, writes=wb)
        op("act", lambda e: e.activation(out_ap, out_ap, AF.Exp, scale=-0.5), reads=wb, writes=wb)

    xt = [P.sbuf(f"xt{i}", [128, D], F32) for i in range(2)]
    xt_b = P.bufs(2, "xt")
    sqs = P.sbuf("sqs", [128, D], BF16)
    sqs_b = P.buf("sqs")
    nst = [P.sbuf(f"nst{i}", [128, 4], F32) for i in range(2)]
    nst_b = P.bufs(2, "nst")
    htmp = P.sbuf("htmp", [128, D], F32)
    htmp_b = P.buf("htmp")
    hb = [P.sbuf(f"hb{i}", [128, D], BF16) for i in range(2)]
    hb_b = P.bufs(2, "hb")
    hT = [P.sbuf(f"hT{i}", [128, 8, 128], BF16) for i in range(2)]
    hT_b = P.bufs(2, "hT")
    cnt = {"x": 0}

    def x_src(t):
        if t < NT:
            return xin.ap()[t * 128:(t + 1) * 128, :]
        return ctxin.ap()[(t - NT) * 128:(t - NT + 1) * 128, :]

    def norm_tile(src_ap, src_reads, sc1, sh, modb, want_f32T=None):
        s = cnt["x"] % 2
        cnt["x"] += 1
        op("act", lambda e: e.activation(sqs[:, :], src_ap, AF.Square, accum_out=nst[s][:, 0:1]),
           reads=src_reads, writes=[sqs_b, nst_b[s]])
        rstd_from_ss(nst[s][:, 0:1], nst[s][:, 1:2], D, [nst_b[s]], [nst_b[s]])
        op("dve", lambda e: e.scalar_tensor_tensor(htmp[:, :], src_ap, nst[s][:, 1:2], sc1[:, :], op0=ALU.mult, op1=ALU.mult),
           reads=src_reads + [nst_b[s], modb], writes=[htmp_b])
        if want_f32T is not None:
            op("pool", lambda e: e.tensor_tensor(want_f32T[0][:, :], htmp[:, :], sh[:, :], op=ALU.add),
               reads=[htmp_b, modb], writes=[want_f32T[1]])
        op("dve", lambda e: e.tensor_tensor(hb[s][:, :], htmp[:, :], sh[:, :], op=ALU.add),
           reads=[htmp_b, modb], writes=[hb_b[s]])
        pt, ptb = pst()
        for kc in range(8):
            op("pe", lambda e, kc=kc: e.transpose(pt[:, kc * 128:(kc + 1) * 128], hb[s][:, kc * 128:(kc + 1) * 128], ident[:, :]),
               reads=[hb_b[s], cb], writes=[ptb], mark=(kc == 7))
        op("act", lambda e: e.copy(hT[s][:, :, :], pt[:, :].rearrange("p (k t) -> p k t", k=8)), reads=[ptb], writes=[hT_b[s]])
        return hT[s], hT_b[s]

    def load_norm(t, sc1, sh, modb):
        s = cnt["x"] % 2
        dma("sp", f"xt{s}", xt[s][:, :], x_src(t), writes=[xt_b[s]])
        return norm_tile(xt[s][:, :], [xt_b[s]], sc1, sh, modb)

    def project(hTt, hTb, wsb, wsb_b, c0, n, ps, psb, off=0):
        for kc in range(8):
            op("pe", lambda e, kc=kc: e.matmul(ps[:, off:off + n], hTt[:, kc, :], wsb[:, kc, c0:c0 + n],
                                              start=(kc == 0), stop=(kc == 7)),
               reads=[hTb, wsb_b], writes=[psb], mark=(kc == 7))

    if not A:
        ntm = NTT if ctx_full else NT
        mixT = P.sbuf("mixT", [128, 8, ntm * 128], BF16)
        mixT_b = P.bufs(ntm, "mixT")
    P.push_scope()
    sc1_l = P.sbuf("sc1_l", [128, D], F32)
    sh_l = P.sbuf("sh_l", [128, D], F32)
    sc1_c = P.sbuf("sc1_c", [128, D], F32)
    sh_c = P.sbuf("sh_c", [128, D], F32)
    modl_b = P.buf("modl")
    modc_b = P.buf("modc")
    load_mod(sh_l, 0, 0, False, modl_b)
    load_mod(sc1_l, 0, 1, True, modl_b)
    load_mod(sh_c, 1, 0, False, modc_b)
    load_mod(sc1_c, 1, 1, True, modc_b)

    def mods_for(t):
        return (sc1_l, sh_l, modl_b) if t < NT else (sc1_c, sh_c, modc_b)

    w_in_v = w_in.ap().rearrange("(kc p) n -> p kc n", p=128)


    P.push_scope()
    wA = P.sbuf("wA", [128, 8, 1280], BF16)
    wA_b = P.buf("wA")
    if A:
        dma("pool", "wA", wA[:, :, 512:768], w_in_v[:, :, 512:768], writes=[wA_b])
    else:
        dma("pool", "wA", wA[:, :, 0:768], w_in_v[:, :, 0:768], writes=[wA_b])
        dma("pool", "wA", wA[:, :, 768:1280], w_in_v[:, :, 2048:2560], writes=[wA_b])
    rope = P.sbuf("rope", [128, NTT, 2, 64], F32)
    rope_b = P.buf("rope")
    dma("sp", "rope", rope[:, :, :, :], rope_d[:, :, :, :], writes=[rope_b])
    gain10 = P.sbuf("gain10", [128, 10, 64], F32)
    gain_b = P.buf("gain")
    dma("sp", "gain", gain10[:, 0:8, :], qkgain[0, :][None, None, :].broadcast_to([128, 8, 64]), writes=[gain_b])
    dma("sp", "gain", gain10[:, 8:10, :], qkgain[1, :][None, None, :].broadcast_to([128, 2, 64]), writes=[gain_b])

    NS = NT + 4
    if not A:
        qT = P.sbuf("qT", [128, NTT, 4, 128], BF16)
        qT_b = P.bufs(NTT, "qT")
        kT = P.sbuf("kT", [128, NS, 128], BF16)
        kT_b = P.bufs(NS, "kT")
        vaug = P.sbuf("vaug", [128, NS, 2, 65], BF16)
        vaug_b = P.bufs(NS, "vaug")
        op("pool", lambda e: e.memset(vaug[:, :, :, 64:65], 1.0), writes=vaug_b)
    sq10 = P.sbuf("sq10", [128, 10, 64], F32)
    sq10_b = P.buf()
    st10 = P.sbuf("st10", [128, 2, 10], F32)
    st10_b = P.buf()
    qn = P.sbuf("qn", [128, 10, 64], F32)
    qn_b = P.buf()
    qg = P.sbuf("qg", [128, 10, 64], F32)
    qg_b = P.buf()
    r1 = P.sbuf("r1", [128, 10, 64], F32)
    r1_b = P.buf()
    r2 = P.sbuf("r2", [128, 10, 64], F32)
    r2_b = P.buf()
    qr = P.sbuf("qr", [128, 640], BF16)
    qr_b = P.buf()
    vtmp = P.sbuf("vtmp", [128, 128], BF16)
    vtmp_b = P.buf()
    ktmp_b = P.buf()

    def slot_of(t):
        return t + 1 if t < NT else NT + 2 + (t - NT)

    def attn_prep(t, hTt, hTb, only_kv):
        nh = 2 if only_kv else 10
        h0 = 8 if only_kv else 0
        pkv, pkvb = psf()
        project(hTt, hTb, wA, wA_b, 512, 256, pkv, pkvb)
        if not only_kv:
            pq, pqb = psf()
            project(hTt, hTb, wA, wA_b, 0, 512, pq, pqb)
            op("act", lambda e: e.activation(sq10[:, 0:8, :], pq[:, :].rearrange("p (h d) -> p h d", h=8), AF.Square),
               reads=[pqb], writes=[sq10_b])
        op("act", lambda e: e.activation(sq10[:, 8:10, :], pkv[:, 0:128].rearrange("p (h d) -> p h d", h=2), AF.Square),
           reads=[pkvb], writes=[sq10_b])
        op("dve", lambda e: e.tensor_reduce(st10[:, 0, h0:10], sq10[:, h0:10, :], axis=AX.X, op=ALU.add),
           reads=[sq10_b], writes=[st10_b])
        rstd_from_ss(st10[:, 0, h0:10], st10[:, 1, h0:10], 64, [st10_b], [st10_b])
        if not only_kv:
            op("dve", lambda e: e.tensor_tensor(qn[:, 0:8, :], pq[:, :].rearrange("p (h d) -> p h d", h=8),
                                                st10[:, 1, 0:8][:, :, None].broadcast_to([128, 8, 64]), op=ALU.mult),
               reads=[pqb, st10_b], writes=[qn_b])
        op("dve", lambda e: e.tensor_tensor(qn[:, 8:10, :], pkv[:, 0:128].rearrange("p (h d) -> p h d", h=2),
                                            st10[:, 1, 8:10][:, :, None].broadcast_to([128, 2, 64]), op=ALU.mult),
           reads=[pkvb, st10_b], writes=[qn_b])
        op("pool", lambda e: e.tensor_tensor(qg[:, h0:10, :], qn[:, h0:10, :], gain10[:, h0:10, :], op=ALU.mult),
           reads=[qn_b, gain_b], writes=[qg_b])
        Ct = rope[:, t, 0, :]
        St = rope[:, t, 1, :]
        op("dve", lambda e: e.tensor_tensor(r1[:, h0:10, :], qg[:, h0:10, :], Ct[:, None, :].broadcast_to([128, nh, 64]), op=ALU.mult),
           reads=[qg_b, rope_b], writes=[r1_b])
        qg5 = qg[:, h0:10, :].rearrange("p h (a f d) -> p h a f d", a=2, f=2)
        r25 = r2[:, h0:10, :].rearrange("p h (a f d) -> p h a f d", a=2, f=2)
        S5 = St.rearrange("p (a f d) -> p a f d", a=2, f=2)
        for f in range(2):
            op("pool", lambda e, f=f: e.tensor_tensor(r25[:, :, :, f, :], qg5[:, :, :, 1 - f, :],
                                                      S5[:, :, f, :][:, None, :, :].broadcast_to([128, nh, 2, 16]), op=ALU.mult),
               reads=[qg_b, rope_b], writes=[r2_b])
        if not only_kv:
            op("dve", lambda e: e.tensor_tensor(qr[:, 0:512].rearrange("p (g k d) -> p k g d", g=4, k=2),
                                                r1[:, 0:8, :].rearrange("p (k g) d -> p k g d", k=2),
                                                r2[:, 0:8, :].rearrange("p (k g) d -> p k g d", k=2), op=ALU.add),
               reads=[r1_b, r2_b], writes=[qr_b])
        op("dve", lambda e: e.tensor_tensor(qr[:, 512:640].rearrange("p (h d) -> p h d", h=2), r1[:, 8:10, :], r2[:, 8:10, :], op=ALU.add),
           reads=[r1_b, r2_b], writes=[qr_b])
        if A:
            which = 0 if t == 0 else 1
            op("act", lambda e: e.copy(vtmp[:, :], pkv[:, 128:256]), reads=[pkvb], writes=[vtmp_b])
            dma("sp", "kbo", kb_o.ap()[which, :, :], qr[:, 512:640], reads=[qr_b])
            dma("sp", "kbo", vb_o.ap()[which, :, :], vtmp[:, :], reads=[vtmp_b])
            return
        sl = slot_of(t)
        op("act", lambda e: e.copy(vaug[:, sl, :, 0:64], pkv[:, 128:256].rearrange("p (h d) -> p h d", h=2)),
           reads=[pkvb], writes=[vaug_b[sl]])
        pt, ptb = pst()
        for g in range(5):
            op("pe", lambda e, g=g: e.transpose(pt[:, g * 128:(g + 1) * 128], qr[:, g * 128:(g + 1) * 128], ident[:, :]),
               reads=[qr_b, cb], writes=[ptb], mark=(g == 4))
        op("act", lambda e: e.copy(qT[:, t, :, :], pt[:, 0:512].rearrange("p (g t) -> p g t", g=4)), reads=[ptb], writes=[qT_b[t]])
        op("act", lambda e: e.copy(kT[:, sl, :], pt[:, 512:640]), reads=[ptb], writes=[kT_b[sl]])

    if not A:
        wsn = P.sbuf("wsn", [128, 4, 128], BF16)
        wsn_b = P.buf()
        dma("pool", "wsn", wsn[:, :, :], gm_ws.ap().rearrange("g t s -> t g s"), writes=[wsn_b])
        wsT = P.sbuf("wsT", [128, 4, 128], BF16)
        wsT_b = P.buf()
        pt, ptb = pst()
        for g in range(4):
            op("pe", lambda e, g=g: e.transpose(pt[:, g * 128:(g + 1) * 128], wsn[:, g, :], ident[:, :]),
               reads=[wsn_b, cb], writes=[ptb], mark=(g == 3))
        op("act", lambda e: e.copy(wsT[:, :, :], pt[:, 0:512].rearrange("p (g t) -> p g t", g=4)), reads=[ptb], writes=[wsT_b])
        gbs = P.sbuf("gbs", [128, 4], F32)
        gbs_b = P.buf()
        dma("sp", "gbs", gbs[:, :], gm_bs.ap().rearrange("g t -> t g"), writes=[gbs_b])
        ggain = P.sbuf("ggain", [128, 256], F32)
        ggain_b = P.buf()
        dma("sp", "ggain", ggain[:, :], gm_gain.ap()[None, :].broadcast_to([128, 256]), writes=[ggain_b])
        gl = P.sbuf("gl", [128, 512], F32)
        gl_b = P.buf()
        gsq = P.sbuf("gsq", [128, 256], F32)
        gsq_b = P.buf()
        gst = P.sbuf("gst", [128, 2, 4], F32)
        gst_b = P.buf()
        gvn = P.sbuf("gvn", [128, 256], F32)
        gvn_b = P.buf()
        gvb = P.sbuf("gvb", [128, 256], BF16)
        gvb_b = P.buf()
        gmo = P.sbuf("gmo", [128, 256], BF16)
        gmo_b = P.buf()

        def mix_transposes(src, src_b, ncol, kc0, t):
            pt, ptb = pst()
            for g in range(ncol):
                op("pe", lambda e, g=g: e.transpose(pt[:, g * 128:(g + 1) * 128], src[:, g * 128:(g + 1) * 128], ident[:, :]),
                   reads=[src_b, cb], writes=[ptb], mark=(g == ncol - 1))
            op("act", lambda e: e.copy(mixT[:, kc0:kc0 + ncol, t * 128:(t + 1) * 128],
                                       pt[:, 0:ncol * 128].rearrange("p (g t) -> p g t", g=ncol)),
               reads=[ptb], writes=[mixT_b[t]])

        def gmlp(t, hTt, hTb):
            pg, pgb = psf()
            project(hTt, hTb, wA, wA_b, 768, 512, pg, pgb)
            op("act", lambda e: e.activation(gl[:, :], pg[:, :], AF.Gelu), reads=[pgb], writes=[gl_b])
            op("act", lambda e: e.activation(gsq[:, :], gl[:, 256:512], AF.Square), reads=[gl_b], writes=[gsq_b])
            op("dve", lambda e: e.tensor_reduce(gst[:, 0, :], gsq[:, :].rearrange("p (g d) -> p g d", g=4), axis=AX.X, op=ALU.add),
               reads=[gsq_b], writes=[gst_b])
            rstd_from_ss(gst[:, 0, :], gst[:, 1, :], 64, [gst_b], [gst_b])
            op("dve", lambda e: e.tensor_tensor(gvn[:, :].rearrange("p (g d) -> p g d", g=4), gl[:, 256:512].rearrange("p (g d) -> p g d", g=4),
                                                gst[:, 1, :][:, :, None].broadcast_to([128, 4, 64]), op=ALU.mult),
               reads=[gl_b, gst_b], writes=[gvn_b])
            op("pool", lambda e: e.tensor_tensor(gvb[:, :], gvn[:, :], ggain[:, :], op=ALU.mult), reads=[gvn_b, ggain_b], writes=[gvb_b])
            pm, pmb = psf()
            for g in range(4):
                op("pe", lambda e, g=g: e.matmul(pm[:, g * 64:(g + 1) * 64], wsT[:, g, :], gvb[:, g * 64:(g + 1) * 64], start=True, stop=True),
                   reads=[wsT_b, gvb_b], writes=[pmb], mark=(g == 3))
            for g in range(4):
                op("dve", lambda e, g=g: e.scalar_tensor_tensor(gmo[:, g * 64:(g + 1) * 64], pm[:, g * 64:(g + 1) * 64], gbs[:, g:g + 1],
                                                                gl[:, g * 64:(g + 1) * 64], op0=ALU.add, op1=ALU.mult),
                   reads=[pmb, gbs_b, gl_b], writes=[gmo_b])
            if t == 0:
                dbgdump("gl", gl[:, :], [128, 512], F32, [gl_b])
                dbgdump("gvb", gvb[:, :], [128, 256], BF16, [gvb_b])
                dbgdump("gmo", gmo[:, :], [128, 256], BF16, [gmo_b])
                dbgdump("gst", gst[:, :, :], [128, 2, 4], F32, [gst_b])
            mix_transposes(gmo, gmo_b, 2, 6, t)

    if A:
        for t in (0, NT - 1):
            hTt, hTb = load_norm(t, *mods_for(t))
            attn_prep(t, hTt, hTb, True)
    else:
        order = list(range(NT, NTT)) + list(range(NT))
        for t in order:
            hTt, hTb = load_norm(t, *mods_for(t))
            attn_prep(t, hTt, hTb, False)
            if t < NT or ctx_full:
                gmlp(t, hTt, hTb)
        hk = P.sbuf("hk", [128, 2, 128], BF16)
        hk_b = P.buf()
        dma("sp", "halo", hk[:, :, :], khalo.ap().rearrange("w t c -> t w c"), writes=[hk_b])
        for w, sl in ((0, 0), (1, NT + 1)):
            dma("sp", "halo", vaug[:, sl, :, 0:64], vhalo.ap()[w, :, :].rearrange("t (h d) -> t h d", h=2), writes=[vaug_b[sl]])
            pt, ptb = pst()
            op("pe", lambda e, w=w: e.transpose(pt[:, 0:128], hk[:, w, :], ident[:, :]), reads=[hk_b, cb], writes=[ptb])
            op("act", lambda e, sl=sl: e.copy(kT[:, sl, :], pt[:, 0:128]), reads=[ptb], writes=[kT_b[sl]])
        am = P.sbuf("am", [128, 4, 128], BF16)
        am_b = P.buf()
        dma("sp", "am", am[:, :, :], amask_d.ap().rearrange("m r a -> r m a"), writes=[am_b])
        snk = P.sbuf("snk", [128, 8], F32)
        snk_b = P.buf()
        dma("sp", "snk", snk[:, :], sink.ap()[None, :].broadcast_to([128, 8]), writes=[snk_b])
        op("act", lambda e: e.activation(snk[:, :], snk[:, :], AF.Exp), reads=[snk_b], writes=[snk_b])
        pT = [P.sbuf(f"pT{i}", [128, 5, 512], BF16) for i in range(2)]
        pT_b = [P.bufs(5, f"pT{i}_") for i in range(2)]
        den = P.sbuf("den", [128, 2, 4], F32)
        den_b = P.buf()
        ao = [P.sbuf(f"ao{i}", [128, 512], BF16) for i in range(2)]
        ao_b = P.bufs(2, "ao")
        acnt = 0
        q_tiles = list(range(NT)) + (list(range(NT, NTT)) if ctx_full else [])
        for t in q_tiles:
            aos, aosb = ao[t % 2], ao_b[t % 2]
            if t < NT:
                blks = [(t, 0 if t == 0 else 1), (t + 1, None), (t + 2, 2 if t == NT - 1 else 3), (NT + 2, None), (NT + 3, None)]
            else:
                blks = [(NT + 2, None), (NT + 3, None)]
            for h2 in range(2):
                pTs, pTsb = pT[acnt % 2], pT_b[acnt % 2]
                acnt += 1
                lo = h2 * 64
                for bi, (sl, mk) in enumerate(blks):
                    ps, psb = psf()
                    op("pe", lambda e, ps=ps, sl=sl: e.matmul(ps[:, :], kT[lo:lo + 64, sl, :], qT[lo:lo + 64, t, :, :].rearrange("p g t -> p (g t)"),
                                                             start=True, stop=True),
                       reads=[kT_b[sl], qT_b[t]], writes=[psb])
                    op("act", lambda e, ps=ps, bi=bi: e.activation(pTs[:, bi, :], ps[:, :], AF.Exp, scale=0.125), reads=[psb], writes=[pTsb[bi]])
                    if mk is not None:
                        op("dve", lambda e, bi=bi, mk=mk: e.tensor_tensor(pTs[:, bi, :].rearrange("p (g t) -> p g t", g=4),
                                                                          pTs[:, bi, :].rearrange("p (g t) -> p g t", g=4),
                                                                          am[:, mk, :][:, None, :].broadcast_to([128, 4, 128]), op=ALU.mult),
                           reads=[pTsb[bi], am_b], writes=[pTsb[bi]])
                po, pob = psf()
                for g in range(4):
                    for bi, (sl, mk) in enumerate(blks):
                        op("pe", lambda e, g=g, bi=bi, sl=sl: e.matmul(po[:, g * 128:g * 128 + 65], pTs[:, bi, g * 128:(g + 1) * 128], vaug[:, sl, h2, :],
                                                                       start=(bi == 0), stop=(bi == len(blks) - 1)),
                           reads=[pTsb[bi], vaug_b[sl]], writes=[pob], mark=(g == 3 and bi == len(blks) - 1))
                po3 = po[:, :].rearrange("p (g c) -> p g c", g=4)
                op("dve", lambda e: e.tensor_tensor(den[:, 0, :], po3[:, :, 64], snk[:, h2 * 4:(h2 + 1) * 4], op=ALU.add),
                   reads=[pob, snk_b], writes=[den_b])
                op("dve", lambda e: e.reciprocal(den[:, 1, :], den[:, 0, :]), reads=[den_b], writes=[den_b])
                op("dve", lambda e: e.tensor_tensor(aos[:, h2 * 256:(h2 + 1) * 256].rearrange("p (g d) -> p g d", g=4), po3[:, :, 0:64],
                                                    den[:, 1, :][:, :, None].broadcast_to([128, 4, 64]), op=ALU.mult),
                   reads=[pob, den_b], writes=[aosb])
            if t == 1:
                dbgdump("pT", pTs[:, :, :], [128, 5, 512], BF16, pTsb)
                dbgdump("aos", aos[:, :], [128, 512], BF16, [aosb])
                dbgdump("den", den[:, :, :], [128, 2, 4], F32, [den_b])
                dbgdump("qT1", qT[:, 1, :, :], [128, 4, 128], BF16, [qT_b[1]])
                dbgdump("kT", kT[:, :, :], [128, NS, 128], BF16, kT_b)
                dbgdump("vaug", vaug[:, :, :, :], [128, NS, 2, 65], BF16, vaug_b)
            mix_transposes(aos, aosb, 4, 0, t)
    P.pop_scope()

    if STOP == "P1":
        P.pop_scope()
        dbg = dout("dbg_mixT", [128, 8, ntm * 128], BF16)
        dma("sp", "dbg", dbg.ap(), mixT[:, :, :], reads=mixT_b)
        return P.finish()
    P.push_scope()
    lbv = P.sbuf("lbv", [128, 2, 256], F32)
    oml = P.sbuf("oml", [128, 2, 256], F32)
    P.push_scope()
    lbt = P.sbuf("lbt", [128, 2, 2, 256], F32)
    lb_b = P.buf("lb")
    dma("sp", "lb", lbt[:, :, :, :], lbnd.ap()[None, :, :, :].broadcast_to([128, 2, 2, 256]), writes=[lb_b])
    op("act", lambda e: e.activation(lbt[:, :, :, :], lbt[:, :, :, :], AF.Exp), reads=[lb_b], writes=[lb_b])
    lbw = P.sbuf("lbw", [128, 4, 2, 256], F32)
    op("dve", lambda e: e.tensor_tensor(lbw[:, 0, :, :], lbt[:, :, 0, :], lbt[:, :, 1, :], op=ALU.add), reads=[lb_b], writes=[lb_b])
    op("dve", lambda e: e.reciprocal(lbw[:, 0, :, :], lbw[:, 0, :, :]), reads=[lb_b], writes=[lb_b])
    op("dve", lambda e: e.tensor_tensor(lbw[:, 1, :, :], lbt[:, :, 0, :], lbw[:, 0, :, :], op=ALU.mult), reads=[lb_b], writes=[lb_b])
    op("dve", lambda e: e.tensor_tensor(lbw[:, 2, :, :], lbt[:, :, 1, :], lbw[:, 0, :, :], op=ALU.mult), reads=[lb_b], writes=[lb_b])
    if layer == 0:
        op("dve", lambda e: e.tensor_tensor(lbv[:, :, :], lbw[:, 1, :, :], lbw[:, 1, :, :], op=ALU.subtract), reads=[lb_b], writes=[lb_b])
    else:
        op("dve", lambda e: e.tensor_tensor(lbw[:, 3, :, :], lbw[:, 1, :, :], lbw[:, 2, :, :], op=ALU.add), reads=[lb_b], writes=[lb_b])
        op("dve", lambda e: e.tensor_tensor(lbv[:, :, :], lbw[:, 3, :, :], lbw[:, 1, :, :], op=ALU.subtract), reads=[lb_b], writes=[lb_b])
    op("dve", lambda e: e.tensor_scalar(oml[:, :, :], lbv[:, :, :], -1.0, 1.0, op0=ALU.mult, op1=ALU.add), reads=[lb_b], writes=[lb_b])
    P.pop_scope()

    h_tiles = list(range(NT)) if A else (list(range(NT, NTT)) + list(range(NT)))
    kt_st = P.sbuf("kt_st", [128, NTT, 512], BF16)
    kt_b = P.bufs(NTT, "kt")
    vh_st = P.sbuf("vh_st", [128, NTT, 256], BF16)
    vh_b = P.bufs(NTT, "vh")
    Est = P.sbuf("Est", [128, NTT, 4, 16], F32)
    E_b = P.bufs(NTT, "E")
    if not A:
        qkT = P.sbuf("qkT", [128, NTT, 8, 128], BF16)
        qkT_b = P.bufs(NTT, "qkT")
        hgate = P.sbuf("hgate", [128, NTT, 256], BF16)
        hgate_b = P.bufs(NTT, "hgate")
        hmask = P.sbuf("hmask", [128, 2, 128], F32)
        hmask_b = P.buf()
        dma("sp", "hmask", hmask[:, :, :], hmask_d.ap().rearrange("d s t -> s d t"), writes=[hmask_b])
    P.push_scope()
    wH = P.sbuf("wH", [128, 8, 1280], BF16)
    wH_b = P.buf("wH")
    if A:
        dma("pool", "wH", wH[:, :, 256:1024], w_in_v[:, :, 1024:1792], writes=[wH_b])
    else:
        dma("pool", "wH", wH[:, :, :], w_in_v[:, :, 768:2048], writes=[wH_b])
    eall = P.sbuf("eall", [128, 1024], F32)
    eall_b = P.buf()
    qh = P.sbuf("qh", [128, 256], F32)
    qh_b = P.buf()
    ff = P.sbuf("ff", [128, 512], F32)
    ff_b = P.buf()
    gln = P.sbuf("gln", [128, 512], F32)
    gln_b = P.buf()
    kk = P.sbuf("kk", [128, 512], F32)
    kk_b = P.buf()
    ea = P.sbuf("ea", [128, 2, 512], F32)
    ea_b = P.buf()
    acl = P.sbuf("acl", [128, 512], F32)
    acl_b = P.buf()
    qt_t = P.sbuf("qt_t", [128, 512], BF16)
    qt_tb = P.buf()

    def hgrn_prep(t, hTt, hTb):
        full = not A
        pf, pfb = psf()
        project(hTt, hTb, wH, wH_b, 256, 512, pf, pfb)
        pi, pib = psf()
        project(hTt, hTb, wH, wH_b, 768, 512 if full else 256, pi, pib)
        op("act", lambda e: e.activation(eall[:, 256:768], pf[:, :], AF.Exp, scale=-1.0), reads=[pfb], writes=[eall_b])
        if full:
            pq, pqb = psf()
            project(hTt, hTb, wH, wH_b, 0, 256, pq, pqb)
            op("act", lambda e: e.activation(eall[:, 0:256], pq[:, 0:256], AF.Exp, scale=-1.0), reads=[pqb], writes=[eall_b])
            op("act", lambda e: e.activation(eall[:, 768:1024], pi[:, 256:512], AF.Exp, scale=-1.0), reads=[pib], writes=[eall_b])
        lo, hi = (0, 1024) if full else (256, 768)
        op("dve", lambda e: e.tensor_scalar_add(eall[:, lo:hi], eall[:, lo:hi], 1.0), reads=[eall_b], writes=[eall_b])
        op("dve", lambda e: e.reciprocal(eall[:, lo:hi], eall[:, lo:hi]), reads=[eall_b], writes=[eall_b])
        if full:
            op("dve", lambda e: e.scalar_tensor_tensor(qh[:, :], pq[:, 0:256], 0.125, eall[:, 0:256], op0=ALU.mult, op1=ALU.mult),
               reads=[pqb, eall_b], writes=[qh_b])
            op("dve", lambda e: e.tensor_tensor(hgate[:, t, :], pi[:, 256:512], eall[:, 768:1024], op=ALU.mult),
               reads=[pib, eall_b], writes=[hgate_b[t]])
        op("act", lambda e: e.copy(vh_st[:, t, :], pi[:, 0:256]), reads=[pib], writes=[vh_b[t]])
        op("dve", lambda e: e.tensor_tensor(ff[:, :], eall[:, 256:768], oml[:, :, :].rearrange("p d k -> p (d k)"), op=ALU.mult),
           reads=[eall_b, lb_b], writes=[ff_b])
        op("dve", lambda e: e.tensor_tensor(ff[:, :], ff[:, :], lbv[:, :, :].rearrange("p d k -> p (d k)"), op=ALU.add),
           reads=[ff_b, lb_b], writes=[ff_b])
        op("act", lambda e: e.activation(gln[:, :], ff[:, :], AF.Ln), reads=[ff_b], writes=[gln_b])
        op("pool", lambda e: e.tensor_scalar(kk[:, :], ff[:, :], -1.0, 1.0, op0=ALU.mult, op1=ALU.add), reads=[ff_b], writes=[kk_b])
        pa, pab = psf()
        for d in range(2):
            op("pe", lambda e, d=d: e.matmul(pa[:, d * 256:(d + 1) * 256], mrel[:, d, :], gln[:, d * 256:(d + 1) * 256], start=True, stop=True),
               reads=[cb, gln_b], writes=[pab], mark=(d == 1))
        pe_, peb = psf()
        for dp in range(4):
            d = dp // 2
            op("pe", lambda e, dp=dp, d=d: e.matmul(pe_[:, dp * 16:dp * 16 + 16], gln[:, dp * 128:(dp + 1) * 128], ind[:, d, :], start=True, stop=True),
               reads=[cb, gln_b], writes=[peb], mark=(dp == 3))
        op("act", lambda e: e.activation(Est[:, t, :, :], pe_[:, 0:64].rearrange("p (a b) -> p a b", a=4), AF.Exp), reads=[peb], writes=[E_b[t]])
        op("dve", lambda e: e.tensor_scalar(acl[:, :], pa[:, :], -40.0, 40.0, op0=ALU.max, op1=ALU.min), reads=[pab], writes=[acl_b])
        op("act", lambda e: e.activation(ea[:, 1, :], acl[:, :], AF.Exp, scale=-1.0), reads=[acl_b], writes=[ea_b])
        op("dve", lambda e: e.tensor_tensor(kt_st[:, t, :], kk[:, :], ea[:, 1, :], op=ALU.mult), reads=[kk_b, ea_b], writes=[kt_b[t]])
        if full:
            op("act", lambda e: e.activation(ea[:, 0, :], acl[:, :], AF.Exp), reads=[acl_b], writes=[ea_b])
            op("dve", lambda e: e.tensor_tensor(qt_t[:, :].rearrange("p (d k) -> p d k", d=2), ea[:, 0, :].rearrange("p (d k) -> p d k", d=2),
                                                qh[:, :][:, None, :].broadcast_to([128, 2, 256]), op=ALU.mult),
               reads=[ea_b, qh_b], writes=[qt_tb])
            pt, ptb = pst()
            for g in range(4):
                op("pe", lambda e, g=g: e.transpose(pt[:, g * 128:(g + 1) * 128], qt_t[:, g * 128:(g + 1) * 128], ident[:, :]),
                   reads=[qt_tb, cb], writes=[ptb], mark=False)
            for g in range(4):
                op("pe", lambda e, g=g: e.transpose(pt[:, (4 + g) * 128:(5 + g) * 128], kt_st[:, t, g * 128:(g + 1) * 128], ident[:, :]),
                   reads=[kt_b[t], cb], writes=[ptb], mark=(g == 3))
            op("act", lambda e: e.copy(qkT[:, t, :, :], pt[:, :].rearrange("p (g t) -> p g t", g=8)), reads=[ptb], writes=[qkT_b[t]])

    for t in h_tiles:
        hTt, hTb = load_norm(t, *mods_for(t))
        hgrn_prep(t, hTt, hTb)
    P.pop_scope()

    if STOP == "P2a":
        return P.finish()
    if not A:
        Shat = P.sbuf("Shat", [128, 2, NCH * NTT, 2, 64], BF16)
        Shat_b = [P.bufs(NCH * NTT, f"Shat{d}_") for d in range(2)]
    P.push_scope()
    S = [P.sbuf(f"S{d}", [128, 2, 64], F32) for d in range(2)]
    S_b = P.bufs(2, "S")
    Tt = [P.sbuf(f"Tt{d}", [128, 2, 64], F32) for d in range(2)]
    Tt_b = P.bufs(2, "Tt")
    KVs = [P.sbuf(f"KVs{i}", [128, 2, 64], F32) for i in range(2)]
    KVs_b = P.bufs(2, "KVs")
    if A:
        Arun = P.sbuf("Arun", [128, 2, 2], F32)
        Arun_b = P.buf()
        op("dve", lambda e: e.memset(Arun[:, :, :], 1.0), writes=[Arun_b])
    kvc = {"n": 0}

    def chain_step(d, t, c, want_hat):
        i = kvc["n"] % 2
        kvc["n"] += 1
        pk, pkb = psf()
        for h in range(4):
            p, j = h // 2, h % 2
            op("pe", lambda e, h=h, p=p, j=j: e.matmul(pk[j * 64:(j + 1) * 64, p * 64:(p + 1) * 64],
                                                      kt_st[c * HC:(c + 1) * HC, t, d * 256 + h * 64: d * 256 + (h + 1) * 64],
                                                      vh_st[c * HC:(c + 1) * HC, t, h * 64:(h + 1) * 64],
                                                      start=True, stop=True, tile_position=(c * HC, j * 64)),
               reads=[kt_b[t], vh_b[t]], writes=[pkb], mark=(h == 3))
        Ed = Est[:, t, d * 2:d * 2 + 2, :]
        op("dve", lambda e: e.tensor_tensor(KVs[i][:, :, :], pk[:, 0:128].rearrange("p (a v) -> p a v", a=2),
                                            Ed[:, :, 3 * c + 2][:, :, None].broadcast_to([128, 2, 64]), op=ALU.mult),
           reads=[pkb, E_b[t]], writes=[KVs_b[i]])
        if want_hat:
            ch = NCH * t + c
            op("pool", lambda e: e.tensor_tensor(Shat[:, d, ch, :, :], S[d][:, :, :],
                                                 Ed[:, :, 3 * c + 0][:, :, None].broadcast_to([128, 2, 64]), op=ALU.mult),
               reads=[S_b[d], E_b[t]], writes=[Shat_b[d][ch]])
        op("dve", lambda e: e.tensor_tensor(Tt[d][:, :, :], S[d][:, :, :], Ed[:, :, 3 * c + 1][:, :, None].broadcast_to([128, 2, 64]), op=ALU.mult),
           reads=[S_b[d], E_b[t]], writes=[Tt_b[d]])
        op("dve", lambda e: e.tensor_tensor(S[d][:, :, :], Tt[d][:, :, :], KVs[i][:, :, :], op=ALU.add),
           reads=[Tt_b[d], KVs_b[i]], writes=[S_b[d]])
        if A:
            op("dve", lambda e: e.tensor_tensor(Arun[:, d, :], Arun[:, d, :], Ed[:, :, 3 * c + 1], op=ALU.mult),
               reads=[Arun_b, E_b[t]], writes=[Arun_b])

    def run_chain(d, tiles, want_hat):
        seq = [(t, c) for t in tiles for c in range(NCH)]
        if d == 1:
            seq = seq[::-1]
        for (t, c) in seq:
            chain_step(d, t, c, want_hat)

    for d in range(2):
        op("dve", lambda e, d=d: e.memset(S[d][:, :, :], 0.0), writes=[S_b[d]])
    if A:
        abt = P.sbuf("abt", [128, 2, 2, 65], F32)
        abt_b = P.buf()
        for d in range(2):
            run_chain(d, list(range(NT)), False)
            op("dve", lambda e, d=d: e.tensor_copy(abt[:, d, :, 0:64], S[d][:, :, :]), reads=[S_b[d]], writes=[abt_b])
            op("dve", lambda e, d=d: e.tensor_copy(abt[:, d, :, 64], Arun[:, d, :]), reads=[Arun_b], writes=[abt_b])
        dma("sp", "abo", ab_o.ap(), abt[:, :, :, :], reads=[abt_b])
        P.pop_scope()
    else:
        abl = P.sbuf("abl", [128, NCORES, 2, 2, 65], F32)
        abl_b = P.buf()
        dma("sp", "abl", abl[:, :, :, :, :], ab_all.ap(), writes=[abl_b])
        rmk = P.sbuf("rmk", [128, 2, NCORES], F32)
        rmk_b = P.buf()
        dma("sp", "rmk", rmk[:, :, :], rmask.ap(), writes=[rmk_b])
        for d in range(2):
            run_chain(d, list(range(NT, NTT)), ctx_full)
            ranks = list(range(NCORES)) if d == 0 else list(range(NCORES))[::-1]
            for r in ranks:
                op("dve", lambda e, d=d, r=r: e.tensor_tensor(Tt[d][:, :, :], S[d][:, :, :],
                                                              abl[:, r, d, :, 64][:, :, None].broadcast_to([128, 2, 64]), op=ALU.mult),
                   reads=[S_b[d], abl_b], writes=[Tt_b[d]])
                op("dve", lambda e, d=d, r=r: e.tensor_tensor(Tt[d][:, :, :], Tt[d][:, :, :], abl[:, r, d, :, 0:64], op=ALU.add),
                   reads=[Tt_b[d], abl_b], writes=[Tt_b[d]])
                op("dve", lambda e, d=d: e.tensor_tensor(Tt[d][:, :, :], Tt[d][:, :, :], S[d][:, :, :], op=ALU.subtract),
                   reads=[Tt_b[d], S_b[d]], writes=[Tt_b[d]])
                op("dve", lambda e, d=d, r=r: e.scalar_tensor_tensor(S[d][:, :, :], Tt[d][:, :, :], rmk[:, d, r:r + 1], S[d][:, :, :],
                                                                     op0=ALU.mult, op1=ALU.add),
                   reads=[Tt_b[d], S_b[d], rmk_b], writes=[S_b[d]])
            run_chain(d, list(range(NT)), True)

        P.pop_scope()
        if STOP == "P2b":
            return P.finish()
        hgn = P.sbuf("hgn", [128, 64], F32)
        hgn_b = P.buf()
        dma("sp", "hgn", hgn[:, :], hgain.ap()[None, :].broadcast_to([128, 64]), writes=[hgn_b])
        scm = [P.sbuf(f"scm{i}", [128, 4, 128], BF16) for i in range(2)]
        scm_b = P.bufs(2, "scm")
        osq = P.sbuf("osq", [128, 256], F32)
        osq_b = P.buf()
        ost = P.sbuf("ost", [128, 2, 4], F32)
        ost_b = P.buf()
        y1 = P.sbuf("y1", [128, 256], F32)
        y1_b = P.buf()
        y2 = P.sbuf("y2", [128, 256], F32)
        y2_b = P.buf()
        yo = P.sbuf("yo", [128, 256], BF16)
        yo_b = P.buf()
        out_tiles = list(range(NT)) + (list(range(NT, NTT)) if ctx_full else [])
        for t in out_tiles:
            if OUTLVL < 1:
                continue
            po, pob = psf()
            sms = []
            for d in range(2):
                pscs = [psf(), psf()]
                for h in range(4):
                    p, j = h // 2, h % 2
                    op("pe", lambda e, h=h, p=p, j=j, d=d: e.matmul(pscs[j][0][:, p * 128:(p + 1) * 128],
                                                                   qkT[j * 64:(j + 1) * 64, t, 4 + d * 2 + p, :],
                                                                   qkT[j * 64:(j + 1) * 64, t, d * 2 + p, :], start=True, stop=True),
                       reads=[qkT_b[t]], writes=[pscs[j][1]], mark=(h >= 2))
                sm, smb = scm[d], scm_b[d]
                sm4 = sm[:, :, :].rearrange("p (a j) t -> p j a t", j=2)
                for j in range(2):
                    op("dve", lambda e, d=d, j=j: e.tensor_tensor(sm4[:, j, :, :], pscs[j][0][:, 0:256].rearrange("p (h t) -> p h t", h=2),
                                                                  hmask[:, d, :][:, None, :].broadcast_to([128, 2, 128]), op=ALU.mult),
                       reads=[pscs[j][1], hmask_b], writes=[smb])
                sms.append((sm, smb))
            for h in range(4):
                p, j = h // 2, h % 2
                for d in range(2):
                    sm, smb = sms[d]
                    op("pe", lambda e, h=h, d=d, sm=sm: e.matmul(po[:, h * 64:(h + 1) * 64], sm[:, h, :], vh_st[:, t, h * 64:(h + 1) * 64],
                                                                 start=(d == 0), stop=False),
                       reads=[smb, vh_b[t]], writes=[pob], mark=False)
                    for c in range(NCH):
                        ch = NCH * t + c
                        op("pe", lambda e, h=h, p=p, j=j, c=c, d=d, ch=ch: e.matmul(
                            po[c * HC:(c + 1) * HC, h * 64:(h + 1) * 64],
                            qkT[j * 64:(j + 1) * 64, t, d * 2 + p, c * HC:(c + 1) * HC],
                            Shat[j * 64:(j + 1) * 64, d, ch, p, :],
                            start=False, stop=(d == 1 and c == NCH - 1), tile_position=(j * 64, c * HC)),
                           reads=[qkT_b[t], Shat_b[d][ch]], writes=[pob], mark=(d == 1 and c == NCH - 1))
            if OUTLVL < 3:
                continue
            op("act", lambda e: e.activation(osq[:, :], po[:, 0:256], AF.Square), reads=[pob], writes=[osq_b])
            op("dve", lambda e: e.tensor_reduce(ost[:, 0, :], osq[:, :].rearrange("p (h d) -> p h d", h=4), axis=AX.X, op=ALU.add),
               reads=[osq_b], writes=[ost_b])
            rstd_from_ss(ost[:, 0, :], ost[:, 1, :], 64, [ost_b], [ost_b])
            op("dve", lambda e: e.tensor_tensor(y1[:, :].rearrange("p (h d) -> p h d", h=4), po[:, 0:256].rearrange("p (h d) -> p h d", h=4),
                                                ost[:, 1, :][:, :, None].broadcast_to([128, 4, 64]), op=ALU.mult),
               reads=[pob, ost_b], writes=[y1_b])
            op("pool", lambda e: e.tensor_tensor(y2[:, :].rearrange("p (h d) -> p h d", h=4), y1[:, :].rearrange("p (h d) -> p h d", h=4),
                                                 hgn[:, :][:, None, :].broadcast_to([128, 4, 64]), op=ALU.mult), reads=[y1_b, hgn_b], writes=[y2_b])
            op("pool", lambda e: e.tensor_tensor(yo[:, :], y2[:, :], hgate[:, t, :], op=ALU.mult), reads=[y2_b, hgate_b[t]], writes=[yo_b])
            if t == 0:
                dbgdump("y1", y1[:, :], [128, 256], F32, [y1_b])
                dbgdump("yo", yo[:, :], [128, 256], BF16, [yo_b])
                dbgdump("osq", osq[:, :], [128, 256], F32, [osq_b])
                dbgdump("qkT0", qkT[:, 0, :, :], [128, 8, 128], BF16, [qkT_b[0]])
                dbgdump("Shat", Shat[:, :, :, :, :], [128, 2, NCH * NTT, 2, 64], BF16, Shat_b[0] + Shat_b[1])
                dbgdump("Est", Est[:, :, :, :], [128, NTT, 4, 16], F32, E_b)
                dbgdump("hgate0", hgate[:, 0, :], [128, 256], BF16, [hgate_b[0]])
                dbgdump("vh0", vh_st[:, 0, :], [128, 256], BF16, [vh_b[0]])
                dbgdump("kt0", kt_st[:, 0, :], [128, 512], BF16, [kt_b[0]])
            mix_transposes(yo, yo_b, 2, 4, t)
    P.pop_scope()
    P.pop_scope()

    if A:
        return P.finish()
    if STOP in ("P1", "P2"):
        dbg = dout("dbg_mixT", [128, 8, ntm * 128], BF16)
        dma("sp", "dbg", dbg.ap(), mixT[:, :, :], reads=mixT_b)
        return P.finish()

    P.push_scope()
    ntf = NTT if ctx_full else NT
    x1 = P.sbuf("x1", [128, ntf, D], F32)
    x1_b = P.bufs(ntf, "x1")
    hT2, hT2_b = mixT, mixT_b
    P.push_scope()
    wo = P.sbuf("wo", [128, 8, D], BF16)
    wo_b = P.buf("wo")
    dma("pool", "wo", wo[:, :, :], w_out.ap().rearrange("(kc p) n -> p kc n", p=128), writes=[wo_b])
    ga_l = P.sbuf("ga_l", [128, D], F32)
    ga_c = P.sbuf("ga_c", [128, D], F32)
    ga_b = P.buf("ga")
    load_mod(ga_l, 0, 2, False, ga_b)
    if ctx_full:
        load_mod(ga_c, 1, 2, False, ga_b)
    otmp = [P.sbuf(f"otmp{i}", [128, 512], F32) for i in range(2)]
    otmp_b = P.bufs(2, "otmp")
    oc = 0
    for t in range(ntf):
        s = cnt["x"] % 2
        cnt["x"] += 1
        dma("sp", f"xt{s}", xt[s][:, :], x_src(t), writes=[xt_b[s]])
        ga = ga_l if t < NT else ga_c
        for half in range(2):
            ps, psb = psf()
            for kc in range(8):
                op("pe", lambda e, kc=kc, half=half, ps=ps: e.matmul(ps[:, :], mixT[:, kc, t * 128:(t + 1) * 128], wo[:, kc, half * 512:(half + 1) * 512],
                                                                  start=(kc == 0), stop=(kc == 7)),
                   reads=[mixT_b[t], wo_b], writes=[psb], mark=(kc == 7))
            ot, otb = otmp[oc % 2], otmp_b[oc % 2]
            oc += 1
            op("dve", lambda e, ps=ps, half=half, ot=ot, ga=ga: e.tensor_tensor(ot[:, :], ps[:, :], ga[:, half * 512:(half + 1) * 512], op=ALU.mult),
               reads=[psb, ga_b], writes=[otb])
            op("pool", lambda e, half=half, ot=ot, s=s: e.tensor_tensor(x1[:, t, half * 512:(half + 1) * 512], ot[:, :], xt[s][:, half * 512:(half + 1) * 512], op=ALU.add),
               reads=[otb, xt_b[s]], writes=[x1_b[t]])
    P.pop_scope()

    gf_l = P.sbuf("gf_l", [128, D], F32)
    mf_b = P.buf("mf")
    load_mod(gf_l, 0, 5, False, mf_b)
    if ctx_full:
        gf_c = P.sbuf("gf_c", [128, D], F32)
        load_mod(gf_c, 1, 5, False, mf_b)
    if moe:
        gate = P.sbuf("gate", [128, NT, N_EXP], F32)
        gate_b = P.bufs(NT, "gate")
    P.push_scope()
    scf_l = P.sbuf("scf_l", [128, D], F32)
    shf_l = P.sbuf("shf_l", [128, D], F32)
    load_mod(shf_l, 0, 3, False, mf_b)
    load_mod(scf_l, 0, 4, True, mf_b)
    if ctx_full:
        scf_c = P.sbuf("scf_c", [128, D], F32)
        shf_c = P.sbuf("shf_c", [128, D], F32)
        load_mod(shf_c, 1, 3, False, mf_b)
        load_mod(scf_c, 1, 4, True, mf_b)
    if moe:
        rt = P.sbuf("rt", [128, 8, N_EXP], F32)
        rt_b = P.buf()
        dma("sp", "rt", rt[:, :, :], router.ap().rearrange("(kc p) e -> p kc e", p=128), writes=[rt_b])
        hf32 = P.sbuf("hf32", [128, D], F32)
        hf32_b = P.buf()
        hTf = P.sbuf("hTf", [128, 8, 128], F32)
        hTf_b = P.buf()
        rs = P.sbuf("rs", [128, 6, 8], F32)
        rs_b = P.buf()
    for t in range(ntf):
        lat = t < NT
        sc1, sh = (scf_l, shf_l) if lat else (scf_c, shf_c)
        hTt, hTb = norm_tile(x1[:, t, :], [x1_b[t]], sc1, sh, mf_b, want_f32T=(hf32, hf32_b) if moe else None)
        op("dve", lambda e, t=t, hTt=hTt: e.tensor_copy(hT2[:, :, t * 128:(t + 1) * 128], hTt[:, :, :]), reads=[hTb], writes=[hT2_b[t]])
        if moe:
            for kc in range(8):
                ps, psb = psf()
                op("pe", lambda e, kc=kc, ps=ps: e.transpose(ps[:, 0:128], hf32[:, kc * 128:(kc + 1) * 128], identf[:, :]), reads=[hf32_b, cb], writes=[psb])
                op("act", lambda e, kc=kc, ps=ps: e.copy(hTf[:, kc, :], ps[:, 0:128]), reads=[psb], writes=[hTf_b])
            pl, plb = psf()
            for kc in range(8):
                op("pe", lambda e, kc=kc: e.matmul(pl[:, 0:8], hTf[:, kc, :], rt[:, kc, :], start=(kc == 0), stop=(kc == 7)),
                   reads=[hTf_b, rt_b], writes=[plb], mark=(kc == 7))
            op("dve", lambda e: e.tensor_copy(rs[:, 0, :], pl[:, 0:8]), reads=[plb], writes=[rs_b])
            op("dve", lambda e: e.max(rs[:, 1, :], rs[:, 0, :]), reads=[rs_b], writes=[rs_b])
            op("dve", lambda e: e.tensor_scalar(rs[:, 2, :], rs[:, 0, :], rs[:, 1, 1:2], None, op0=ALU.is_ge), reads=[rs_b], writes=[rs_b])
            op("dve", lambda e: e.tensor_scalar(rs[:, 3, :], rs[:, 0, :], rs[:, 1, 0:1], None, op0=ALU.subtract), reads=[rs_b], writes=[rs_b])
            op("act", lambda e: e.activation(rs[:, 3, :], rs[:, 3, :], AF.Exp), reads=[rs_b], writes=[rs_b])
            op("dve", lambda e: e.tensor_tensor(rs[:, 4, :], rs[:, 3, :], rs[:, 2, :], op=ALU.mult), reads=[rs_b], writes=[rs_b])
            op("dve", lambda e: e.tensor_reduce(rs[:, 5, 0:1], rs[:, 4, :], axis=AX.X, op=ALU.add), reads=[rs_b], writes=[rs_b])
            op("dve", lambda e: e.reciprocal(rs[:, 5, 1:2], rs[:, 5, 0:1]), reads=[rs_b], writes=[rs_b])
            op("dve", lambda e, t=t: e.tensor_scalar(gate[:, t, :], rs[:, 4, :], rs[:, 5, 1:2], None, op0=ALU.mult), reads=[rs_b], writes=[gate_b[t]])

    P.pop_scope()
    ntok = ntf * 128
    groups = [(n0, min(512, ntok - n0)) for n0 in range(0, ntok, 512)]
    if moe:
        units = [(e, q * 7, 7) for e in range(N_EXP) for q in range(4)]
    else:
        units = [(0, 0, 6), (0, 6, 6), (0, 12, 5), (0, 17, 5)]
    UMAX = 7
    act = P.sbuf("actb", [128, UMAX, ntok], BF16)
    act_b = [[P.buf() for _ in groups] for _ in range(UMAX)]
    NWG = 3
    wgs = [P.sbuf(f"wgs{i}", [128, 2, 8, 128], BF16) for i in range(NWG)]
    wgs_b = P.bufs(NWG, "wgs")
    NWD = 8
    wds = [P.sbuf(f"wds{i}", [128, D], BF16) for i in range(NWD)]
    wds_b = P.bufs(NWD, "wds")
    sil = [P.sbuf(f"sil{i}", [128, 512], F32) for i in range(2)]
    sil_b = P.bufs(2, "sil")
    ftmp = [P.sbuf(f"ftmp{i}", [128, 512], F32) for i in range(2)]
    ftmp_b = P.bufs(2, "ftmp")
    wgc = wdc = sc_ = fc = 0
    for (ex, j0, nj) in units:
        wdslots = []
        for jj in range(nj):
            j = j0 + jj
            ws, wsb = wgs[wgc % NWG], wgs_b[wgc % NWG]
            dsn = f"wgs{wgc % NWG}"
            wgc += 1
            dma("pool", dsn, ws[:, 0, :, :], wg.ap()[ex, :, j * 128:(j + 1) * 128].rearrange("(kc p) n -> p kc n", p=128), writes=[wsb])
            dma("pool", dsn, ws[:, 1, :, :], wu.ap()[ex, :, j * 128:(j + 1) * 128].rearrange("(kc p) n -> p kc n", p=128), writes=[wsb])
            wdi = wdc % NWD
            wdc += 1
            dma("pool", f"wds{wdi}", wds[wdi][:, :], wd.ap()[ex, j * 128:(j + 1) * 128, :], writes=[wds_b[wdi]])
            wdslots.append(wdi)
            for gi, (n0, nn) in enumerate(groups):
                pg, pgb = psf()
                pu, pub = psf()
                for kc in range(8):
                    op("pe", lambda e, kc=kc, pg=pg, ws=ws, n0=n0, nn=nn: e.matmul(pg[:, 0:nn], ws[:, 0, kc, :], hT2[:, kc, n0:n0 + nn], start=(kc == 0), stop=(kc == 7)),
                       reads=[wsb] + hT2_b[n0 // 128:(n0 + nn) // 128], writes=[pgb], mark=(kc == 7))
                for kc in range(8):
                    op("pe", lambda e, kc=kc, pu=pu, ws=ws, n0=n0, nn=nn: e.matmul(pu[:, 0:nn], ws[:, 1, kc, :], hT2[:, kc, n0:n0 + nn], start=(kc == 0), stop=(kc == 7)),
                       reads=[wsb] + hT2_b[n0 // 128:(n0 + nn) // 128], writes=[pub], mark=(kc == 7))
                sl_, slb = sil[sc_ % 2], sil_b[sc_ % 2]
                sc_ += 1
                op("act", lambda e, pg=pg, sl_=sl_, nn=nn: e.activation(sl_[:, 0:nn], pg[:, 0:nn], AF.Silu), reads=[pgb], writes=[slb])
                op("dve", lambda e, pu=pu, sl_=sl_, nn=nn, n0=n0, jj=jj: e.tensor_tensor(act[:, jj, n0:n0 + nn], sl_[:, 0:nn], pu[:, 0:nn], op=ALU.mult),
                   reads=[slb, pub], writes=[act_b[jj][gi]])
        for t in range(ntf):
            lat = t < NT
            gf = gf_l if lat else gf_c
            for half in range(2):
                pd, pdb = psf()
                for jj in range(nj):
                    op("pe", lambda e, jj=jj, pd=pd, t=t, half=half, wdi=wdslots[jj]: e.matmul(pd[:, :], act[:, jj, t * 128:(t + 1) * 128],
                                                                                              wds[wdi][:, half * 512:(half + 1) * 512],
                                                                                              start=(jj == 0), stop=(jj == nj - 1)),
                       reads=[act_b[jj][t // 4], wds_b[wdslots[jj]]], writes=[pdb], mark=(jj == nj - 1))
                ft, ftb = ftmp[fc % 2], ftmp_b[fc % 2]
                fc += 1
                if moe:
                    op("dve", lambda e, pd=pd, ft=ft, t=t, half=half, ex=ex, gf=gf: e.scalar_tensor_tensor(
                        ft[:, :], pd[:, :], gate[:, t, ex:ex + 1], gf[:, half * 512:(half + 1) * 512], op0=ALU.mult, op1=ALU.mult),
                       reads=[pdb, gate_b[t], mf_b], writes=[ftb])
                else:
                    op("dve", lambda e, pd=pd, ft=ft, half=half, gf=gf: e.tensor_tensor(ft[:, :], pd[:, :], gf[:, half * 512:(half + 1) * 512], op=ALU.mult),
                       reads=[pdb, mf_b], writes=[ftb])
                op("dve", lambda e, ft=ft, t=t, half=half: e.tensor_tensor(x1[:, t, half * 512:(half + 1) * 512], x1[:, t, half * 512:(half + 1) * 512], ft[:, :], op=ALU.add),
                   reads=[ftb, x1_b[t]], writes=[x1_b[t]])
    for t in range(ntf):
        if t < NT:
            dma("sp", "xo", xout.ap()[t * 128:(t + 1) * 128, :], x1[:, t, :], reads=[x1_b[t]])
        else:
            dma("sp", "xo", ctxout.ap()[(t - NT) * 128:(t - NT + 1) * 128, :], x1[:, t, :], reads=[x1_b[t]])
    P.pop_scope()
    return P.finish()


_CACHE = {}
STOP = None
DBG = False
import os
NOINTER = os.environ.get('NOINTER') == '1'
OUTLVL = int(os.environ.get('OUTLVL', '9'))


def _prog(kind, layer):
    key = (kind, layer)
    if key not in _CACHE:
        _CACHE[key] = build(kind, layer)
    return _CACHE[key]


def _run(kind, layer, in_maps):
    res = run_bass_kernel_spmd(_prog(kind, layer), in_maps, core_ids=list(range(NCORES)))
    return res.results


def kernel(x, c, ctx, c_ctx, w_ada, b_ada, w_in, w_out, q_norm_gain, k_norm_gain, attn_sink,
           hgrn_lower_bound, hgrn_out_gain, gmlp_w_s, gmlp_b_s, gmlp_norm_gain,
           ffn_w_gate, ffn_w_up, ffn_w_down, moe_router, moe_w_gate, moe_w_up, moe_w_down):
    f32 = np.float32
    bf = ml_dtypes.bfloat16
    A_ = lambda a: np.ascontiguousarray(np.asarray(a), dtype=f32)
    x = A_(x)[0]
    ctx_cur = A_(ctx)[0]
    cvec = np.stack([A_(c)[0], A_(c_ctx)])
    cs = _consts()
    ropes = [_rope_tables(i) for i in range(NCORES)]
    amasks = [_amask(i) for i in range(NCORES)]
    xs = [np.ascontiguousarray(x[i * TOK:(i + 1) * TOK]) for i in range(NCORES)]
    for layer in range(2):
        common = dict(ctxin=ctx_cur, cvec=cvec, w_ada=A_(w_ada)[layer], b_ada=A_(b_ada)[layer], w_in=A_(w_in)[layer],
                      qkgain=np.stack([A_(q_norm_gain)[layer], A_(k_norm_gain)[layer]]),
                      lbnd=A_(hgrn_lower_bound), ident=cs["ident"], identf=cs["identf"], mrel=cs["mrel"], ind=cs["ind"])
        resA = _run("A", layer, [dict(common, xin=xs[i], rope=ropes[i]) for i in range(NCORES)])
        ab_all = np.ascontiguousarray(np.stack([resA[i]["ab"] for i in range(NCORES)], axis=1))
        zero_t = np.zeros((128, 128), bf)
        in_maps = []
        moe = layer % 2 == 1
        for i in range(NCORES):
            kh = np.stack([resA[i - 1]["kb"][1] if i > 0 else zero_t, resA[i + 1]["kb"][0] if i < NCORES - 1 else zero_t])
            vh = np.stack([resA[i - 1]["vb"][1] if i > 0 else zero_t, resA[i + 1]["vb"][0] if i < NCORES - 1 else zero_t])
            rm = np.zeros((128, 2, NCORES), f32)
            rm[:, 0, :i] = 1.0
            rm[:, 1, i + 1:] = 1.0
            m = dict(common, xin=xs[i], rope=ropes[i], hmask=cs["hmask"], amask=amasks[i], khalo=kh.astype(bf), vhalo=vh.astype(bf),
                     ab_all=ab_all, rmask=rm, w_out=A_(w_out)[layer], sink=A_(attn_sink)[layer], hgain=A_(hgrn_out_gain)[layer],
                     gm_ws=A_(gmlp_w_s)[layer], gm_bs=A_(gmlp_b_s)[layer], gm_gain=A_(gmlp_norm_gain)[layer])
            if moe:
                m.update(router=A_(moe_router)[layer // 2], wg=A_(moe_w_gate)[layer // 2], wu=A_(moe_w_up)[layer // 2], wd=A_(moe_w_down)[layer // 2])
            else:
                m.update(wg=A_(ffn_w_gate)[layer // 2][None], wu=A_(ffn_w_up)[layer // 2][None], wd=A_(ffn_w_down)[layer // 2][None])
            in_maps.append(m)
        resB = _run("B", layer, in_maps)
        xs = [np.ascontiguousarray(resB[i]["xout"]) for i in range(NCORES)]
        if layer == 0:
            ctx_cur = np.ascontiguousarray(resB[0]["ctxout"])
    return np.concatenate(xs, axis=0)[None].astype(f32)
```
